# Optimizing a Trainium2 kernel written in Bass

```python
import jax, jax.numpy as jnp
from jax import lax
import numpy as np


D_MODEL = 1024
BATCH = 2
SEQ = 8192
DEPTH = 2

N_MIXERS = 2
ATT_HEADS = 8
ATT_HEAD_DIM = D_MODEL // ATT_HEADS
MOBA_BLOCK = 256
MOBA_TOPK = 3
MOBA_QUERY_CHUNK = 32
MLSTM_HEADS = 8
MLSTM_DV = D_MODEL // MLSTM_HEADS
MLSTM_DQK = MLSTM_DV // 2
MLSTM_CHUNK = 64
GATE_SOFTCAP = 15.0
D_FF = 4 * D_MODEL
RMS_EPS = 1e-6
N_ATTN_LAYERS = (DEPTH + N_MIXERS - 1) // N_MIXERS
N_MLSTM_LAYERS = DEPTH // N_MIXERS

kernel_name = "hybrid_moba_mlstm_sandwich_block"


def rms_norm(x, gain):
    xf = x.astype(jnp.float32)
    y = xf * lax.rsqrt(jnp.mean(xf * xf, axis=-1, keepdims=True) + RMS_EPS)
    return (y * gain.astype(jnp.float32)).astype(x.dtype)


def alibi_slopes(n_heads):
    return jnp.exp2(-8.0 * jnp.arange(1, n_heads + 1, dtype=jnp.float32) / n_heads)


def soft_cap(z):
    return GATE_SOFTCAP * jnp.tanh(z / GATE_SOFTCAP)


def moba_attention(x, w_qkv, w_o):
    bsz, seq, _ = x.shape
    H, dh, blk, qlen = ATT_HEADS, ATT_HEAD_DIM, MOBA_BLOCK, MOBA_QUERY_CHUNK
    f32 = jnp.float32
    q, k, v = jnp.split(x @ w_qkv, 3, axis=-1)

    def to_heads(t):
        return t.reshape(bsz, seq, H, dh).transpose(0, 2, 1, 3)

    q = to_heads(q) * (dh ** -0.5)
    k = to_heads(k)
    v = to_heads(v)
    n_blk = -(-seq // blk)
    pad = n_blk * blk - seq
    k_blk = jnp.pad(k, ((0, 0), (0, 0), (0, pad), (0, 0))).reshape(bsz, H, n_blk, blk, dh)
    v_blk = jnp.pad(v, ((0, 0), (0, 0), (0, pad), (0, 0))).reshape(bsz, H, n_blk, blk, dh)

    k_mean = jnp.mean(k_blk.astype(f32), axis=3)
    gate = jnp.einsum('bhsd,bhnd->bhsn', q.astype(f32), k_mean)
    n_past = jnp.arange(seq) // blk
    is_past = jnp.arange(n_blk)[None, :] < n_past[:, None]
    gate = jnp.where(is_past, gate, -jnp.inf)
    top = min(MOBA_TOPK, n_blk)
    _, sel = lax.top_k(gate, top)
    sel = sel.astype(jnp.int32)

    n_chunk = seq // qlen
    q_c = jnp.moveaxis(q.reshape(bsz, H, n_chunk, qlen, dh), 2, 0)
    sel_c = jnp.moveaxis(sel.reshape(bsz, H, n_chunk, qlen, top), 2, 0)
    slopes = alibi_slopes(H)
    b_idx = jnp.arange(bsz)[:, None, None, None]
    h_idx = jnp.arange(H)[None, :, None, None]
    offs = jnp.arange(blk)

    def attend_chunk(args):
        c, q_blk, sel_blk = args
        t = c * qlen + jnp.arange(qlen)
        own = (c * qlen) // blk
        k_own = lax.dynamic_index_in_dim(k_blk, own, axis=2, keepdims=False)
        v_own = lax.dynamic_index_in_dim(v_blk, own, axis=2, keepdims=False)
        dist_own = (t[:, None] - (own * blk + offs)[None, :]).astype(f32)
        s_own = jnp.einsum('bhqd,bhkd->bhqk', q_blk, k_own).astype(f32) - slopes[:, None, None] * dist_own
        s_own = jnp.where(dist_own >= 0, s_own, -jnp.inf)
        k_sel = k_blk[b_idx, h_idx, sel_blk]
        v_sel = v_blk[b_idx, h_idx, sel_blk]
        dist_sel = (t[:, None, None] - (sel_blk[..., None] * blk + offs)).astype(f32)
        s_sel = jnp.einsum('bhqd,bhqjkd->bhqjk', q_blk, k_sel).astype(f32) - slopes[:, None, None, None] * dist_sel
        slot_ok = jnp.arange(top)[None, :] < (t // blk)[:, None]
        s_sel = jnp.where(slot_ok[:, :, None], s_sel, -jnp.inf)
        scores = jnp.concatenate([s_own, s_sel.reshape(bsz, H, qlen, top * blk)], axis=-1)
        p = jax.nn.softmax(scores, axis=-1).astype(v_blk.dtype)
        p_own = p[..., :blk]
        p_sel = p[..., blk:].reshape(bsz, H, qlen, top, blk)
        return (jnp.einsum('bhqk,bhkd->bhqd', p_own, v_own)
                + jnp.einsum('bhqjk,bhqjkd->bhqd', p_sel, v_sel))

    o = lax.map(attend_chunk, (jnp.arange(n_chunk), q_c, sel_c))
    o = o.transpose(1, 0, 3, 2, 4).reshape(bsz, seq, H * dh)
    return o @ w_o


def mlstm_chunkwise(q, k, v, log_i, log_f):
    bsz, H, seq, dqk = q.shape
    dv = v.shape[-1]
    L = MLSTM_CHUNK
    n_chunk = seq // L

    def chunks(t):
        return jnp.moveaxis(t.reshape(bsz, H, n_chunk, L, *t.shape[3:]), 2, 0)

    causal = jnp.tril(jnp.ones((L, L), dtype=bool))

    def step(carry, inp):
        C, n, m = carry
        qc, kc, vc, ic, fc = inp
        b = jnp.cumsum(fc, axis=-1)
        log_d = jnp.where(causal, b[..., :, None] - b[..., None, :] + ic[..., None, :], -jnp.inf)
        log_inter = b + m[..., None]
        m_t = jnp.maximum(log_inter, jnp.max(log_d, axis=-1))
        d = jnp.exp(log_d - m_t[..., None])
        w_inter = jnp.exp(log_inter - m_t)
        s = jnp.einsum('bhtd,bhsd->bhts', qc, kc) * d
        num = (w_inter[..., None] * jnp.einsum('bhtd,bhdv->bhtv', qc, C)
               + jnp.einsum('bhts,bhsv->bhtv', s, vc))
        den = w_inter * jnp.einsum('bhtd,bhd->bht', qc, n) + jnp.sum(s, axis=-1)
        h = num / jnp.maximum(jnp.abs(den), jnp.exp(-m_t))[..., None]
        b_end = b[..., -1]
        log_w = b_end[..., None] - b + ic
        m_new = jnp.maximum(b_end + m, jnp.max(log_w, axis=-1))
        decay = jnp.exp(b_end + m - m_new)
        w = jnp.exp(log_w - m_new[..., None])
        C = decay[..., None, None] * C + jnp.einsum('bhs,bhsd,bhsv->bhdv', w, kc, vc)
        n = decay[..., None] * n + jnp.einsum('bhs,bhsd->bhd', w, kc)
        return (C, n, m_new), h

    f32 = jnp.float32
    init = (jnp.zeros((bsz, H, dqk, dv), f32), jnp.zeros((bsz, H, dqk), f32), jnp.zeros((bsz, H), f32))
    _, h = lax.scan(step, init, (chunks(q), chunks(k), chunks(v), chunks(log_i), chunks(log_f)))
    return jnp.moveaxis(h, 0, 2).reshape(bsz, H, seq, dv)


def mlstm_mixer(x, w_in, b_gates, norm_h, w_out):
    bsz, seq, _ = x.shape
    H, dqk, dv = MLSTM_HEADS, MLSTM_DQK, MLSTM_DV
    f32 = jnp.float32
    proj = x @ w_in
    q, k, v, o_pre, gate_pre = jnp.split(
        proj, [H * dqk, 2 * H * dqk, 2 * H * dqk + H * dv, 2 * H * dqk + 2 * H * dv], axis=-1)

    def to_heads(t, dim):
        return t.reshape(bsz, seq, H, dim).transpose(0, 2, 1, 3).astype(f32)

    q = to_heads(q, dqk)
    k = to_heads(k, dqk) * (dqk ** -0.5)
    v = to_heads(v, dv)
    gates = soft_cap(gate_pre.astype(f32) + b_gates.astype(f32)).transpose(0, 2, 1)
    log_i = gates[:, :H]
    log_f = jax.nn.log_sigmoid(gates[:, H:])
    h = mlstm_chunkwise(q, k, v, log_i, log_f)
    h = h * lax.rsqrt(jnp.mean(h * h, axis=-1, keepdims=True) + RMS_EPS)
    h = h.transpose(0, 2, 1, 3).reshape(bsz, seq, H * dv) * norm_h.astype(f32)
    y = (jax.nn.sigmoid(o_pre.astype(f32)) * h).astype(x.dtype)
    return y @ w_out


def squared_relu_mlp(x, w_up, w_down):
    return jnp.square(jax.nn.relu(x @ w_up)) @ w_down


def setup_inputs(seed: int = 0) -> dict:
    key = jax.random.key(seed)
    ks = jax.random.split(key, 16)
    f32 = jnp.float32

    def dense(k, shape):
        return jax.random.normal(k, shape, f32) * shape[-2] ** -0.5

    def gain(k, shape):
        return 1.0 + 0.05 * jax.random.normal(k, shape, f32)

    H = MLSTM_HEADS
    mlstm_in = 2 * H * MLSTM_DQK + 2 * H * MLSTM_DV + 2 * H
    i_bias = 0.1 * jax.random.normal(ks[13], (N_MLSTM_LAYERS, H), f32)
    f_bias = 3.0 + 0.5 * jax.random.normal(ks[14], (N_MLSTM_LAYERS, H), f32)
    return {
        "x": jax.random.normal(ks[0], (BATCH, SEQ, D_MODEL), f32),
        "norm_mix_pre": gain(ks[1], (DEPTH, D_MODEL)),
        "norm_mix_post": gain(ks[2], (DEPTH, D_MODEL)),
        "norm_ffn_pre": gain(ks[3], (DEPTH, D_MODEL)),
        "norm_ffn_post": gain(ks[4], (DEPTH, D_MODEL)),
        "w_up": dense(ks[5], (DEPTH, D_MODEL, D_FF)),
        "w_down": dense(ks[6], (DEPTH, D_FF, D_MODEL)),
        "attn_w_qkv": dense(ks[7], (N_ATTN_LAYERS, D_MODEL, 3 * ATT_HEADS * ATT_HEAD_DIM)),
        "attn_w_o": dense(ks[8], (N_ATTN_LAYERS, ATT_HEADS * ATT_HEAD_DIM, D_MODEL)),
        "mlstm_w_in": dense(ks[9], (N_MLSTM_LAYERS, D_MODEL, mlstm_in)),
        "mlstm_b_gates": jnp.concatenate([i_bias, f_bias], axis=-1),
        "mlstm_norm_h": gain(ks[10], (N_MLSTM_LAYERS, H * MLSTM_DV)),
        "mlstm_w_out": dense(ks[11], (N_MLSTM_LAYERS, H * MLSTM_DV, D_MODEL)),
    }


def reference(x, norm_mix_pre, norm_mix_post, norm_ffn_pre, norm_ffn_post, w_up, w_down,
              attn_w_qkv, attn_w_o, mlstm_w_in, mlstm_b_gates, mlstm_norm_h, mlstm_w_out):
    h = x
    for layer in range(DEPTH):
        j = layer // N_MIXERS
        u = rms_norm(h, norm_mix_pre[layer])
        if layer % N_MIXERS == 0:
            u = moba_attention(u, attn_w_qkv[j], attn_w_o[j])
        else:
            u = mlstm_mixer(u, mlstm_w_in[j], mlstm_b_gates[j], mlstm_norm_h[j], mlstm_w_out[j])
        h = h + rms_norm(u, norm_mix_post[layer])
        u = squared_relu_mlp(rms_norm(h, norm_ffn_pre[layer]), w_up[layer], w_down[layer])
        h = h + rms_norm(u, norm_ffn_post[layer])
    return h
```

```python
import numpy as np
import ml_dtypes
import concourse.bass as bass
import concourse.mybir as mybir
from concourse.bass_utils import run_bass_kernel_spmd

F32 = mybir.dt.float32
BF16 = mybir.dt.bfloat16
AF = mybir.ActivationFunctionType
ALU = mybir.AluOpType
AX = mybir.AxisListType
NPBF = ml_dtypes.bfloat16

NCORE = 8
import os
STQ = "pool"
D = 1024
SEQ = 8192
NB = 2
TOK = 2048
DFF = 4096
EPS = 1e-6
SLOPES = [2.0 ** (-(h + 1)) for h in range(8)]


class Buf:
    __slots__ = ("name", "w", "r", "dsem", "dcount", "excl")

    def __init__(self, name, excl=False):
        self.name = name
        self.excl = excl
        self.w = None
        self.r = []
        self.dsem = None
        self.dcount = 0


class _Eng:
    def __init__(self, name, sem):
        self.name = name
        self.sem = sem
        self.count = 0
        self.seen = {}
        self.ops = []
        self.pend_r = []
        self.pend_w = []


class Sched:
    ENGS = ("sync", "act", "dve", "pe", "pool")

    def __init__(self, nc):
        self.nc = nc
        self.e = {n: _Eng(n, nc.alloc_semaphore(name="s_" + n)) for n in self.ENGS}
        self.nsem = len(self.ENGS)
        self.final = []

    def _waits(self, E, reads, writes):
        deps = []
        for b in reads:
            if b.w is not None:
                deps.append(b.w)
        for b in writes:
            if b.w is not None:
                deps.append(b.w)
            deps.extend(b.r)
        waits = []
        for (sem, val, src) in deps:
            if src == E.name and E.name == "pe":
                continue
            key = id(sem)
            if E.seen.get(key, 0) >= val:
                continue
            E.seen[key] = val
            waits.append((sem, val))
        return waits

    def op(self, eng, fn, reads=(), writes=(), inc=True):
        E = self.e[eng]
        ex = [b for b in reads if b.excl]
        if ex:
            reads = [b for b in reads if not b.excl]
            writes = list(writes) + [b for b in ex if b not in writes]
        waits = self._waits(E, reads, writes)
        if inc:
            E.count += 1
            tok = (E.sem, E.count, eng)
            E.ops.append((waits, fn, (E.sem, 1)))
            rs = list(reads) + E.pend_r
            ws = list(writes) + E.pend_w
            E.pend_r = []
            E.pend_w = []
            for b in rs:
                b.r.append(tok)
            for b in ws:
                b.w = tok
                b.r = []
            return tok
        E.ops.append((waits, fn, None))
        E.pend_r.extend(reads)
        E.pend_w.extend(writes)
        return None

    def dma(self, eng, out, in_, sbuf, reads=(), writes=(), final=False, **kw):
        E = self.e[eng]
        waits = self._waits(E, reads, writes)
        if sbuf.dsem is None:
            sbuf.dsem = {}
            sbuf.dcount = {}
        if eng not in sbuf.dsem:
            sbuf.dsem[eng] = self.nc.alloc_semaphore(name="d%d" % self.nsem)
            sbuf.dcount[eng] = 0
            self.nsem += 1
        sbuf.dcount[eng] += 16
        dsem = sbuf.dsem[eng]
        tok = (dsem, sbuf.dcount[eng], "dma")
        E.ops.append((waits, (lambda e, o=out, i=in_, k=kw: e.dma_start(out=o, in_=i, **k)), (dsem, 16)))
        for b in reads:
            b.r.append(tok)
        for b in writes:
            b.w = tok
            b.r = []
        if final:
            self.final.append(tok)
        return tok

    def check(self):
        vals = {}
        pos = {n: 0 for n in self.ENGS}
        prog = True
        while prog:
            prog = False
            for n in self.ENGS:
                ops = self.e[n].ops
                while pos[n] < len(ops):
                    waits, fn, inc = ops[pos[n]]
                    if any(vals.get(id(sem), 0) < val for (sem, val) in waits):
                        break
                    if inc is not None:
                        vals[id(inc[0])] = vals.get(id(inc[0]), 0) + inc[1]
                    pos[n] += 1
                    prog = True
        for n in self.ENGS:
            if pos[n] < len(self.e[n].ops):
                waits, fn, inc = self.e[n].ops[pos[n]]
                raise RuntimeError("deadlock: engine %s stuck at op %d/%d waits %s" % (
                    n, pos[n], len(self.e[n].ops), [(str(sem), val, vals.get(id(sem), 0)) for sem, val in waits]))
        for (sem, val, _) in self.final:
            assert vals.get(id(sem), 0) >= val
        print("sched check ok:", {n: len(self.e[n].ops) for n in self.ENGS}, "nsem", self.nsem)

    def emit(self):
        self.check()
        nc = self.nc
        best = {}
        for (sem, val, _) in self.final:
            k = id(sem)
            if k not in best or best[k][1] < val:
                best[k] = (sem, val)
        fw = list(best.values())

        def run(engobj, E, extra=()):
            for (waits, fn, inc) in E.ops:
                for (sem, val) in waits:
                    engobj.wait_ge(sem, val)
                ins = fn(engobj)
                if inc is not None:
                    ins.then_inc(inc[0], inc[1])
            for (sem, val) in extra:
                engobj.wait_ge(sem, val)

        with nc.Block() as block:
            @block.sync
            def _(e):
                run(e, self.e["sync"], fw)

            @block.scalar
            def _(e):
                run(e, self.e["act"])

            @block.vector
            def _(e):
                run(e, self.e["dve"])

            @block.tensor
            def _(e):
                run(e, self.e["pe"])

            @block.gpsimd
            def _(e):
                run(e, self.e["pool"], fw)


class T:
    def __init__(self, t, b):
        self.t = t
        self.b = b

    def __getitem__(self, k):
        return self.t[k]


class KB:
    def __init__(self):
        self.nc = bass.Bass("TRN2", target_bir_lowering=False)
        self.S = Sched(self.nc)
        self.n = 0

    def sb(self, shape, dtype, name=None):
        name = (name or "t") + "_%d" % self.n
        self.n += 1
        return T(self.nc.alloc_sbuf_tensor(name, list(shape), dtype), Buf(name))

    def ps(self, shape, dtype=F32, name=None):
        name = (name or "p") + "_%d" % self.n
        self.n += 1
        return T(self.nc.alloc_psum_tensor(name, list(shape), dtype), Buf(name, excl=True))

    def din(self, name, shape, dtype):
        return self.nc.dram_tensor(name, list(shape), dtype, kind="ExternalInput").ap()

    def dout(self, name, shape, dtype):
        return self.nc.dram_tensor(name, list(shape), dtype, kind="ExternalOutput").ap()


def load_gain_cols(kb, g_dram, scale=None):
    S = kb.S
    gt = kb.sb([128, 8], F32, "gt")
    S.dma("sync", gt[:], g_dram.rearrange("(c p) -> p c", p=128), gt.b, writes=[gt.b], allow_slow_non_contiguous=True)
    if scale is not None:
        gs = kb.sb([128, 8], F32, "gs")
        S.op("dve", lambda e: e.tensor_scalar(out=gs[:], in0=gt[:], scalar1=float(scale), scalar2=None, op0=ALU.mult),
             reads=[gt.b], writes=[gs.b])
        return gt, gs
    return gt


def load_bcast(kb, g_dram, n):
    t = kb.sb([128, n], F32, "gbc")
    kb.S.dma("sync", t[:], g_dram.partition_broadcast(128), t.b, writes=[t.b])
    return t


class WeightLoader:
    def __init__(self, kb, stage_cols=1024):
        self.kb = kb
        self.sc = stage_cols
        self.stage = [kb.sb([128, stage_cols], F32, "wst") for _ in range(2)]
        self.i = 0
        self.engs = ("pool", "dve")

    def load(self, w_dram, K, N, segs=None):
        kb = self.kb
        S = kb.S
        KC = K // 128
        wb = kb.nc.alloc_sbuf_tensor("wb_%d" % kb.n, [128, KC, N], BF16)
        kb.n += 1
        bufs = [Buf("wb%d_%d" % (kb.n, c)) for c in range(KC)]
        if segs is None:
            segs = [(0, N, None)]
        pieces = []
        for (c0, c1, sc) in segs:
            a = c0
            while a < c1:
                b = min(a + self.sc, c1)
                pieces.append((a, b, sc))
                a = b
        for c in range(KC):
            for (a, b, sc) in pieces:
                st = self.stage[self.i % 2]
                eng = self.engs[self.i % 2]
                self.i += 1
                S.dma("sync", st[:, 0:b - a], w_dram[c * 128:(c + 1) * 128, a:b], st.b, writes=[st.b])
                if sc is None:
                    S.op(eng, lambda e, st=st, a=a, b=b, c=c: e.tensor_copy(out=wb[:, c, a:b], in_=st[:, 0:b - a]),
                         reads=[st.b], writes=[bufs[c]])
                else:
                    S.op(eng, lambda e, st=st, a=a, b=b, c=c, sc=sc: e.tensor_scalar(
                        out=wb[:, c, a:b], in0=st[:, 0:b - a], scalar1=sc[:, c:c + 1], scalar2=None, op0=ALU.mult),
                        reads=[st.b, sc.b], writes=[bufs[c]])
        return wb, bufs


class NormT:
    def __init__(self, kb, ident):
        self.kb = kb
        self.ident = ident
        self.xb = [kb.sb([128, 1024], BF16, "xb") for _ in range(2)]
        self.pT = [kb.ps([128, 1024], BF16, "pT") for _ in range(2)]
        self.ss = [kb.sb([128, 1], F32, "ss") for _ in range(2)]
        self.rs = [kb.sb([128, 1], F32, "rs") for _ in range(2)]
        self.i = 0

    def rstd(self, src_ap, src_buf, junk, ss, rs):
        S = self.kb.S
        S.op("act", lambda e: e.activation(out=junk[:], in_=src_ap, func=AF.Square, accum_out=ss[:]),
             reads=[src_buf], writes=[junk.b, ss.b])
        S.op("act", lambda e: e.activation(out=rs[:], in_=ss[:], func=AF.Ln, scale=1.0 / 1024, bias=EPS),
             reads=[ss.b], writes=[rs.b])
        S.op("act", lambda e: e.activation(out=rs[:], in_=rs[:], func=AF.Exp, scale=-0.5),
             reads=[rs.b], writes=[rs.b])

    def run(self, src_ap, src_buf, xT, col0, norm=True, src_is_bf16=False):
        S = self.kb.S
        k = self.i % 2
        self.i += 1
        xb, pT, ss, rs = self.xb[k], self.pT[k], self.ss[k], self.rs[k]
        if src_is_bf16:
            src_t = None
        if norm:
            self.rstd(src_ap, src_buf, xb, ss, rs)
            S.op("act", lambda e: e.activation(out=xb[:], in_=src_ap, func=AF.Copy, scale=rs[:]),
                 reads=[src_buf, rs.b], writes=[xb.b])
            tin, tb = xb, xb.b
        elif src_is_bf16:
            tin, tb = None, src_buf
        else:
            S.op("act", lambda e: e.activation(out=xb[:], in_=src_ap, func=AF.Copy), reads=[src_buf], writes=[xb.b])
            tin, tb = xb, xb.b
        for c in range(8):
            if tin is None:
                ap_in = src_ap[:, c * 128:(c + 1) * 128]
            else:
                ap_in = tin[:, c * 128:(c + 1) * 128]
            S.op("pe", lambda e, c=c, ap_in=ap_in: e.transpose(out=pT[:, c * 128:(c + 1) * 128], in_=ap_in, identity=self.ident[:]),
                 reads=[tb, self.ident.b], writes=[pT.b], inc=(c == 7))
        S.op("dve", lambda e: e.tensor_copy(out=xT[:, :, col0:col0 + 128], in_=pT[:].rearrange("p (c t) -> p c t", c=8)),
             reads=[pT.b], writes=[xT.b])


def post_norm_residual(kb, nt, u_ps, gbc, h_tile, tmp):
    S = kb.S
    k = nt.i % 2
    nt.i += 1
    ss, rs = nt.ss[k], nt.rs[k]
    nt.rstd(u_ps[:], u_ps.b, tmp, ss, rs)
    S.op("dve", lambda e: e.scalar_tensor_tensor(out=tmp[:], in0=u_ps[:], scalar=rs[:], in1=gbc[:], op0=ALU.mult, op1=ALU.mult),
         reads=[u_ps.b, rs.b, gbc.b], writes=[tmp.b])
    S.op("pool", lambda e: e.tensor_tensor(out=h_tile[:], in0=h_tile[:], in1=tmp[:], op=ALU.add),
         reads=[h_tile.b, tmp.b], writes=[h_tile.b])


def load_ident(kb, ident_d):
    idt = kb.sb([128, 128], BF16, "ident")
    kb.S.dma("sync", idt[:], ident_d, idt.b, writes=[idt.b])
    return idt


def build_k1():
    kb = KB()
    S = kb.S
    x = kb.din("x", [TOK, D], F32)
    g = kb.din("g", [D], F32)
    w = kb.din("w", [D, 3072], F32)
    ident_d = kb.din("ident", [128, 128], BF16)
    qT = kb.dout("qT", [8, 128, TOK], BF16)
    kT = kb.dout("kT", [8, 128, TOK], BF16)
    v = kb.dout("v", [TOK, D], BF16)
    ksum = None

    idt = load_ident(kb, ident_d)
    gt, gs = load_gain_cols(kb, g, scale=128.0 ** -0.5)
    wl = WeightLoader(kb)
    wb, wbufs = wl.load(w, D, 3072, segs=[(0, 1024, gs), (1024, 3072, gt)])
    nt = NormT(kb, idt)
    xt = [kb.sb([128, D], F32, "xt") for _ in range(2)]
    xnT = [kb.sb([128, 8, 512], BF16, "xnT") for _ in range(2)]
    pq = [kb.ps([128, 512], F32, "pq") for _ in range(2)]
    pv = [kb.ps([128, 512], F32, "pv") for _ in range(2)]
    ob = [kb.sb([128, 512], BF16, "ob") for _ in range(4)]
    ks = kb.sb([128, 8, 8], F32, "ks")
    n_ob = 0
    for gi in range(4):
        xT = xnT[gi % 2]
        for ti in range(4):
            t0 = gi * 512 + ti * 128
            xs = xt[(gi * 4 + ti) % 2]
            S.dma("sync", xs[:], x[t0:t0 + 128, :], xs.b, writes=[xs.b])
            nt.run(xs[:], xs.b, xT, ti * 128, norm=True)
        for nch in range(16):
            ps = pq[nch % 2]
            for c in range(8):
                S.op("pe", lambda e, c=c, nch=nch, ps=ps, xT=xT: e.matmul(
                    out=ps[:], lhsT=wb[:, c, nch * 128:(nch + 1) * 128], rhs=xT[:, c, :], start=(c == 0), stop=(c == 7)),
                    reads=[wbufs[c], xT.b], writes=[ps.b], inc=(c == 7))
            o = ob[n_ob % 4]
            n_ob += 1
            S.op("act", lambda e, o=o, ps=ps: e.activation(out=o[:], in_=ps[:], func=AF.Copy), reads=[ps.b], writes=[o.b])
            if nch >= 8:
                hk = nch - 8
                if False: S.op("dve", lambda e, ps=ps, hk=hk, gi=gi: e.tensor_reduce(
                    out=ks[:, hk, gi * 2:gi * 2 + 2], in_=ps[:].rearrange("p (b k) -> p b k", b=2), axis=AX.X, op=ALU.add),
                    reads=[ps.b], writes=[ks.b])
                dst = kT[hk, :, gi * 512:(gi + 1) * 512]
            else:
                dst = qT[nch, :, gi * 512:(gi + 1) * 512]
            S.dma(STQ, dst, o[:], o.b, reads=[o.b], final=True)
        for ti in range(4):
            for half in range(2):
                ps = pv[half]
                for c in range(8):
                    S.op("pe", lambda e, c=c, ti=ti, half=half, ps=ps, xT=xT: e.matmul(
                        out=ps[:], lhsT=xT[:, c, ti * 128:(ti + 1) * 128], rhs=wb[:, c, 2048 + half * 512:2048 + (half + 1) * 512],
                        start=(c == 0), stop=(c == 7)),
                        reads=[wbufs[c], xT.b], writes=[ps.b], inc=(c == 7))
                o = ob[n_ob % 4]
                n_ob += 1
                S.op("dve", lambda e, o=o, ps=ps: e.tensor_copy(out=o[:], in_=ps[:]), reads=[ps.b], writes=[o.b])
                t0 = gi * 512 + ti * 128
                S.dma(STQ, v[t0:t0 + 128, half * 512:(half + 1) * 512], o[:], o.b, reads=[o.b], final=True)
    if False: S.dma(STQ, ksum.rearrange("h d b -> d h b"), ks[:], ks.b, reads=[ks.b], final=True, allow_slow_non_contiguous=True)
    S.emit()
    return kb.nc


def build_k3a(mode):
    kb = KB()
    S = kb.S
    if mode == "attn":
        a_in = kb.din("a_in", [TOK, D], F32)
    else:
        a_in = kb.din("a_in", [TOK, D], F32)
        opre = kb.din("opre", [TOK, D], F32)
    h_in = kb.din("h_in", [TOK, D], F32)
    w = kb.din("w", [D, D], F32)
    gpost = kb.din("gpost", [D], F32)
    ident_d = kb.din("ident", [128, 128], BF16)
    h_out = kb.dout("h_out", [TOK, D], F32)

    idt = load_ident(kb, ident_d)
    gbc = load_bcast(kb, gpost, D)
    wl = WeightLoader(kb)
    wb, wbufs = wl.load(w, D, D)
    nt = NormT(kb, idt)
    at = [kb.sb([128, D], F32, "at") for _ in range(2)]
    if mode != "attn":
        ot = [kb.sb([128, D], F32, "ot") for _ in range(2)]
    ht = [kb.sb([128, D], F32, "ht") for _ in range(2)]
    tmp = [kb.sb([128, D], F32, "tmp") for _ in range(2)]
    aT = [kb.sb([128, 8, 128], BF16, "aT") for _ in range(2)]
    pu = [kb.ps([128, D], F32, "pu") for _ in range(2)]
    for ti in range(TOK // 128):
        k = ti % 2
        t0 = ti * 128
        S.dma("sync", at[k][:], a_in[t0:t0 + 128, :], at[k].b, writes=[at[k].b])
        S.dma("sync", ht[k][:], h_in[t0:t0 + 128, :], ht[k].b, writes=[ht[k].b])
        if mode == "attn":
            nt.run(at[k][:], at[k].b, aT[k], 0, norm=False)
        else:
            S.dma("sync", ot[k][:], opre[t0:t0 + 128, :], ot[k].b, writes=[ot[k].b])
            S.op("act", lambda e, k=k: e.activation(out=ot[k][:], in_=ot[k][:], func=AF.Sigmoid), reads=[ot[k].b], writes=[ot[k].b])
            S.op("dve", lambda e, k=k: e.tensor_tensor(out=at[k][:], in0=at[k][:], in1=ot[k][:], op=ALU.mult),
                 reads=[at[k].b, ot[k].b], writes=[at[k].b])
            nt.run(at[k][:], at[k].b, aT[k], 0, norm=False)
        ps = pu[k]
        for half in range(2):
            for c in range(8):
                S.op("pe", lambda e, c=c, half=half, ps=ps, k=k: e.matmul(
                    out=ps[:, half * 512:(half + 1) * 512], lhsT=aT[k][:, c, :], rhs=wb[:, c, half * 512:(half + 1) * 512],
                    start=(c == 0), stop=(c == 7)),
                    reads=[wbufs[c], aT[k].b], writes=[ps.b], inc=(c == 7 and half == 1))
        post_norm_residual(kb, nt, ps, gbc, ht[k], tmp[k])
        S.dma(STQ, h_out[t0:t0 + 128, :], ht[k][:], ht[k].b, reads=[ht[k].b], final=True)
    S.emit()
    return kb.nc


def build_k3b():
    kb = KB()
    S = kb.S
    h_in = kb.din("h_in", [TOK, D], F32)
    gpre = kb.din("gpre", [D], F32)
    w_up = kb.din("w_up", [D, DFF], F32)
    w_dn = kb.din("w_dn", [DFF, D], F32)
    gpost = kb.din("gpost", [D], F32)
    ident_d = kb.din("ident", [128, 128], BF16)
    h_out = kb.dout("h_out", [TOK, D], F32)

    idt = load_ident(kb, ident_d)
    gbc = load_bcast(kb, gpost, D)
    gt = load_gain_cols(kb, gpre)
    wl = WeightLoader(kb)
    wu, wubufs = wl.load(w_up, D, DFF, segs=[(0, DFF, gt)])
    wd, wdbufs = wl.load(w_dn, DFF, D)
    nt = NormT(kb, idt)
    G = 256
    ht = [kb.sb([128, D], F32, "ht") for _ in range(4)]
    tmp = [kb.sb([128, D], F32, "tmp") for _ in range(2)]
    xnT = [kb.sb([128, 8, G], BF16, "xnT") for _ in range(2)]
    hidT = kb.nc.alloc_sbuf_tensor("hidT", [128, 32, G], BF16)
    hbufs = [Buf("hid%d" % f) for f in range(32)]
    pup = [kb.ps([128, G], F32, "pup") for _ in range(2)]
    rtmp = [kb.sb([128, G], F32, "rtmp") for _ in range(4)]
    pdn = [kb.ps([128, D], F32, "pdn") for _ in range(2)]
    for gi in range(TOK // G):
        xT = xnT[gi % 2]
        hts = []
        for ti in range(2):
            t0 = gi * G + ti * 128
            hs = ht[(gi * 2 + ti) % 4]
            hts.append(hs)
            S.dma("sync", hs[:], h_in[t0:t0 + 128, :], hs.b, writes=[hs.b])
            nt.run(hs[:], hs.b, xT, ti * 128, norm=True)
        for f in range(32):
            ps = pup[f % 2]
            for c in range(8):
                S.op("pe", lambda e, c=c, f=f, ps=ps, xT=xT: e.matmul(
                    out=ps[:], lhsT=wu[:, c, f * 128:(f + 1) * 128], rhs=xT[:, c, :], start=(c == 0), stop=(c == 7)),
                    reads=[wubufs[c], xT.b], writes=[ps.b], inc=(c == 7))
            rt = rtmp[f % 4]
            S.op("act", lambda e, rt=rt, ps=ps: e.activation(out=rt[:], in_=ps[:], func=AF.Relu),
                 reads=[ps.b], writes=[rt.b])
            S.op("dve" if f % 2 == 0 else "pool", lambda e, f=f, rt=rt: e.tensor_tensor(out=hidT[:, f, :], in0=rt[:], in1=rt[:], op=ALU.mult),
                 reads=[rt.b], writes=[hbufs[f]])
        for ti in range(2):
            ps = pdn[ti]
            for half in range(2):
                for f in range(32):
                    S.op("pe", lambda e, f=f, half=half, ps=ps, ti=ti: e.matmul(
                        out=ps[:, half * 512:(half + 1) * 512], lhsT=hidT[:, f, ti * 128:(ti + 1) * 128],
                        rhs=wd[:, f, half * 512:(half + 1) * 512], start=(f == 0), stop=(f == 31)),
                        reads=[wdbufs[f], hbufs[f]], writes=[ps.b], inc=(f == 31 and half == 1))
            post_norm_residual(kb, nt, ps, gbc, hts[ti], tmp[ti])
            t0 = gi * G + ti * 128
            S.dma(STQ, h_out[t0:t0 + 128, :], hts[ti][:], hts[ti].b, reads=[hts[ti].b], final=True)
    S.emit()
    return kb.nc


def build_k2():
    kb = KB()
    S = kb.S
    qT_d = kb.din("qT", [2, 128, SEQ], BF16)
    kT_d = kb.din("kT", [2, 128, SEQ], BF16)
    v_d = kb.din("v", [2, 128, 64, 129], BF16)
    cbias_d = kb.din("cbias", [128, 2, 2], F32)
    cj_d = kb.din("cj", [128, 2, 32], F32)
    cmb_d = kb.din("cmb", [128, 2, 64], F32)
    cown_d = kb.din("cown", [128, 2, 2], F32)
    tri_d = kb.din("tri", [128, 128], BF16)
    o_d = kb.dout("o", [2, SEQ, 128], F32)

    def ld(dram, shape, dt, name):
        t = kb.sb(shape, dt, name)
        S.dma("sync", t[:], dram, t.b, writes=[t.b])
        return t
    cbias = ld(cbias_d, [128, 2, 2], F32, "cbias")
    cj = ld(cj_d, [128, 2, 32], F32, "cj")
    cmb = ld(cmb_d, [128, 2, 64], F32, "cmb")
    cown = ld(cown_d, [128, 2, 2], F32, "cown")
    tri = ld(tri_d, [128, 128], BF16, "tri")
    qT = [kb.sb([128, SEQ], BF16, "qT") for _ in range(2)]
    kT = [kb.sb([128, SEQ], BF16, "kT") for _ in range(2)]
    vt = [kb.sb([128, 64, 129], BF16, "vt") for _ in range(2)]
    for hh in range(2):
        for part in range(4):
            cs = slice(part * 2048, (part + 1) * 2048)
            S.dma("sync", kT[hh][:, cs], kT_d[hh, :, cs], kT[hh].b, writes=[kT[hh].b])
            S.dma("sync", qT[hh][:, cs], qT_d[hh, :, cs], qT[hh].b, writes=[qT[hh].b])
            ts = slice(part * 16, (part + 1) * 16)
            S.dma("sync", vt[hh][:, ts, :], v_d[hh, :, ts, :], vt[hh].b, writes=[vt[hh].b])
    km = kb.sb([128, 32], F32, "km")
    kjunk = kb.sb([128, 256], BF16, "kjunk")
    kmb = kb.sb([128, 32], BF16, "kmb")
    gsb = kb.sb([128, 32], F32, "gsb")
    m8 = kb.sb([128, 8], F32, "m8")
    sel = kb.sb([128, 32], F32, "sel")
    Mt = [kb.sb([128, 32], F32, "M") for _ in range(4)]
    acc = [kb.sb([128, 129], F32, "acc") for _ in range(4)]
    rc = [kb.sb([128, 1], F32, "rc") for _ in range(2)]
    ot = [kb.sb([128, 128], F32, "ot") for _ in range(4)]
    pg = kb.ps([128, 512], F32, "pg")
    def halves(n, nm):
        out = []
        for i in range(n):
            full = kb.ps([128, 512], F32, nm)
            out.append(T(full.t[:, 0:256], Buf(nm + "a%d" % i)))
            out.append(T(full.t[:, 256:512], Buf(nm + "b%d" % i)))
        return out
    pss = [kb.ps([128, 256], F32, "pss") for _ in range(3)]
    pos = [kb.ps([128, 256], F32, "pos") for _ in range(4)]
    pts = [kb.sb([128, 256], BF16, "pt") for _ in range(6)]
    cnt = {"ps": 0, "pt": 0, "po": 0, "ot": 0}

    def nxt(lst, key):
        r = lst[cnt[key] % len(lst)]
        cnt[key] += 1
        return r

    for hh in range(2):
        q_, k_, v_ = qT[hh], kT[hh], vt[hh]
        for j in range(32):
            S.op("act", lambda e, k_=k_, j=j: e.activation(out=kjunk[:], in_=k_[:, j * 256:(j + 1) * 256], func=AF.Copy, scale=1.0 / 256,
                                                        accum_out=km[:, j:j + 1]),
                 reads=[k_.b], writes=[kjunk.b, km.b])
        S.op("dve", lambda e: e.tensor_copy(out=kmb[:], in_=km[:]), reads=[km.b], writes=[kmb.b])
        S.op("pool", lambda e: e.memset(gsb[:], -3.0e38), writes=[gsb.b])
        for B in range(32):
            qs = slice(256 * B, 256 * B + 256)
            Ms = []
            for a in range(2):
                qt = 2 * B + a
                M = Mt[(B % 2) * 2 + a]
                Ms.append(M)
                if B >= 1:
                    S.op("act", lambda e, M=M, B=B, hh=hh, qt=qt: e.activation(
                        out=M[:, 0:B], in_=cj[:, hh, 0:B], func=AF.Exp, bias=cmb[:, hh, qt:qt + 1]),
                        reads=[cj.b, cmb.b], writes=[M.b])
                if B > 3:
                    S.op("pe", lambda e, q_=q_, qt=qt: e.matmul(out=pg[:, 0:32], lhsT=q_[:, qt * 128:(qt + 1) * 128], rhs=kmb[:, 0:32], start=True, stop=True),
                         reads=[q_.b, kmb.b], writes=[pg.b])
                    S.op("dve", lambda e, B=B: e.tensor_copy(out=gsb[:, 0:B], in_=pg[:, 0:B]), reads=[pg.b], writes=[gsb.b])
                    S.op("dve", lambda e: e.max(out=m8[:], in_=gsb[:]), reads=[gsb.b], writes=[m8.b])
                    S.op("dve", lambda e, B=B: e.tensor_scalar(out=sel[:, 0:B], in0=gsb[:, 0:B], scalar1=m8[:, 2:3], scalar2=None, op0=ALU.is_ge),
                         reads=[gsb.b, m8.b], writes=[sel.b])
                    S.op("dve", lambda e, M=M, B=B: e.tensor_tensor(out=M[:, 0:B], in0=M[:, 0:B], in1=sel[:, 0:B], op=ALU.mult),
                         reads=[M.b, sel.b], writes=[M.b])
            accs = [acc[(B % 2) * 2 + a] for a in range(2)]
            own = []
            for t in range(2):
                kt = 2 * B + t
                ps = nxt(pss, "ps")
                S.op("pe", lambda e, ps=ps, k_=k_, q_=q_, kt=kt, qs=qs: e.matmul(
                    out=ps[:, 0:256], lhsT=k_[:, kt * 128:(kt + 1) * 128], rhs=q_[:, qs], start=True, stop=True),
                    reads=[k_.b, q_.b], writes=[ps.b])
                pt = nxt(pts, "pt")
                S.op("act", lambda e, pt=pt, ps=ps, hh=hh: e.activation(out=pt[:, 0:256], in_=ps[:, 0:256], func=AF.Exp, bias=cbias[:, hh, 1:2]),
                     reads=[ps.b, cbias.b], writes=[pt.b])
                own.append(pt)
            S.op("pool", lambda e, p=own[0]: e.tensor_tensor(out=p[:, 0:128], in0=p[:, 0:128], in1=tri[:], op=ALU.mult),
                 reads=[own[0].b, tri.b], writes=[own[0].b])
            S.op("pool", lambda e, p=own[1]: e.tensor_tensor(out=p[:, 128:256], in0=p[:, 128:256], in1=tri[:], op=ALU.mult),
                 reads=[own[1].b, tri.b], writes=[own[1].b])
            po = nxt(pos, "po")
            S.op("pe", lambda e, po=po, p=own[0], v_=v_, B=B: e.matmul(out=po[:, 0:129], lhsT=p[:, 0:128], rhs=v_[:, 2 * B, :], start=True, stop=True),
                 reads=[own[0].b, v_.b], writes=[po.b])
            S.op("dve", lambda e, po=po, A=accs[0], hh=hh: e.tensor_scalar(out=A[:], in0=po[:, 0:129], scalar1=cown[:, hh, 0:1], scalar2=None, op0=ALU.mult),
                 reads=[po.b, cown.b], writes=[accs[0].b])
            po = nxt(pos, "po")
            S.op("pe", lambda e, po=po, p=own[0], v_=v_, B=B: e.matmul(out=po[:, 0:129], lhsT=p[:, 128:256], rhs=v_[:, 2 * B, :], start=True, stop=True),
                 reads=[own[0].b, v_.b], writes=[po.b])
            S.op("dve", lambda e, po=po, A=accs[1], hh=hh: e.tensor_scalar(out=A[:], in0=po[:, 0:129], scalar1=cown[:, hh, 1:2], scalar2=None, op0=ALU.mult),
                 reads=[po.b, cown.b], writes=[accs[1].b])
            po = nxt(pos, "po")
            S.op("pe", lambda e, po=po, p=own[1], v_=v_, B=B: e.matmul(out=po[:, 0:129], lhsT=p[:, 128:256], rhs=v_[:, 2 * B + 1, :], start=True, stop=True),
                 reads=[own[1].b, v_.b], writes=[po.b])
            S.op("dve", lambda e, po=po, A=accs[1], hh=hh: e.scalar_tensor_tensor(
                out=A[:], in0=po[:, 0:129], scalar=cown[:, hh, 0:1], in1=A[:], op0=ALU.mult, op1=ALU.add),
                reads=[po.b, cown.b, accs[1].b], writes=[accs[1].b])
            for j in range(B):
                pt2 = []
                for t in range(2):
                    kt = 2 * j + t
                    ps = nxt(pss, "ps")
                    S.op("pe", lambda e, ps=ps, k_=k_, q_=q_, kt=kt, qs=qs: e.matmul(
                        out=ps[:, 0:256], lhsT=k_[:, kt * 128:(kt + 1) * 128], rhs=q_[:, qs], start=True, stop=True),
                        reads=[k_.b, q_.b], writes=[ps.b])
                    pt = nxt(pts, "pt")
                    S.op("act", lambda e, pt=pt, ps=ps, hh=hh, t=t: e.activation(out=pt[:, 0:256], in_=ps[:, 0:256], func=AF.Exp, bias=cbias[:, hh, t:t + 1]),
                         reads=[ps.b, cbias.b], writes=[pt.b])
                    pt2.append(pt)
                for a in range(2):
                    po = nxt(pos, "po")
                    for t in range(2):
                        S.op("pe", lambda e, po=po, p=pt2[t], v_=v_, a=a, j=j, t=t: e.matmul(
                            out=po[:, 0:129], lhsT=p[:, a * 128:(a + 1) * 128], rhs=v_[:, 2 * j + t, :], start=(t == 0), stop=(t == 1)),
                            reads=[pt2[t].b, v_.b], writes=[po.b], inc=(t == 1))
                    S.op("dve", lambda e, po=po, A=accs[a], M=Ms[a], j=j: e.scalar_tensor_tensor(
                        out=A[:], in0=po[:, 0:129], scalar=M[:, j:j + 1], in1=A[:], op0=ALU.mult, op1=ALU.add),
                        reads=[po.b, Ms[a].b, accs[a].b], writes=[accs[a].b])
            for a in range(2):
                qt = 2 * B + a
                r = rc[a]
                o_ = nxt(ot, "ot")
                S.op("dve", lambda e, r=r, A=accs[a]: e.reciprocal(out=r[:], in_=A[:, 128:129]), reads=[accs[a].b], writes=[r.b])
                S.op("dve", lambda e, r=r, A=accs[a], o_=o_: e.tensor_scalar(out=o_[:], in0=A[:, 0:128], scalar1=r[:], scalar2=None, op0=ALU.mult),
                     reads=[accs[a].b, r.b], writes=[o_.b])
                S.dma(STQ, o_d[hh, qt * 128:(qt + 1) * 128, :], o_[:], o_.b, reads=[o_.b], final=True)
    S.emit()
    return kb.nc


def k2_consts(heads):
    p = np.arange(128, dtype=np.float64)
    cbias = np.zeros((128, 2, 2)); cj = np.zeros((128, 2, 32)); cmb = np.zeros((128, 2, 64)); cown = np.zeros((128, 2, 2))
    for i, h in enumerate(heads):
        sl = SLOPES[h]
        cbias[:, i, 0] = sl * (p - 255)
        cbias[:, i, 1] = sl * (p - 127)
        cj[:, i, :] = sl * 256.0 * np.arange(32)[None, :]
        cmb[:, i, :] = -sl * (128.0 * np.arange(64)[None, :] + p[:, None] - 255)
        cown[:, i, 0] = np.exp(-sl * (p - 127))
        cown[:, i, 1] = np.exp(-sl * (p + 1))
    tri = (np.arange(128)[:, None] <= np.arange(128)[None, :]).astype(NPBF)
    return {"cbias": cbias.astype(np.float32), "cj": cj.astype(np.float32), "cmb": cmb.astype(np.float32),
            "cown": cown.astype(np.float32), "tri": tri}


def build_k4():
    kb = KB()
    S = kb.S
    NIN = 3088
    x = kb.din("x", [TOK, D], F32)
    g = kb.din("g", [D], F32)
    w = kb.din("w", [D, NIN], F32)
    ident_d = kb.din("ident", [128, 128], BF16)
    qT = kb.dout("qT", [4, 128, TOK], BF16)
    kT = kb.dout("kT", [4, 128, TOK], BF16)
    ktok = kb.dout("ktok", [TOK, 512], BF16)
    v = kb.dout("v", [TOK, D], BF16)
    opre = kb.dout("opre", [TOK, D], F32)
    gates = kb.dout("gatesT", [16, TOK], F32)

    idt = load_ident(kb, ident_d)
    gt, gs = load_gain_cols(kb, g, scale=64.0 ** -0.5)
    wl = WeightLoader(kb)
    wb, wbufs = wl.load(w, D, NIN, segs=[(0, 512, gt), (512, 1024, gs), (1024, NIN, gt)])
    nt = NormT(kb, idt)
    xt = [kb.sb([128, D], F32, "xt") for _ in range(2)]
    xnT = [kb.sb([128, 8, 512], BF16, "xnT") for _ in range(2)]
    pq = [kb.ps([128, 512], F32, "pq") for _ in range(2)]
    pv = [kb.ps([128, 512], F32, "pv") for _ in range(2)]
    ob = [kb.sb([128, 512], BF16, "ob") for _ in range(4)]
    of = [kb.sb([128, 512], F32, "of") for _ in range(3)]
    ogs = [kb.sb([16, 512], F32, "og") for _ in range(2)]
    n_ob = 0
    n_of = 0
    n_pv = 0
    for gi in range(4):
        xT = xnT[gi % 2]
        for ti in range(4):
            t0 = gi * 512 + ti * 128
            xs = xt[(gi * 4 + ti) % 2]
            S.dma("sync", xs[:], x[t0:t0 + 128, :], xs.b, writes=[xs.b])
            nt.run(xs[:], xs.b, xT, ti * 128, norm=True)
        for nch in range(8):
            ps = pq[nch % 2]
            for c in range(8):
                S.op("pe", lambda e, c=c, nch=nch, ps=ps, xT=xT: e.matmul(
                    out=ps[:], lhsT=wb[:, c, nch * 128:(nch + 1) * 128], rhs=xT[:, c, :], start=(c == 0), stop=(c == 7)),
                    reads=[wbufs[c], xT.b], writes=[ps.b], inc=(c == 7))
            o = ob[n_ob % 4]
            n_ob += 1
            S.op("act", lambda e, o=o, ps=ps: e.activation(out=o[:], in_=ps[:], func=AF.Copy), reads=[ps.b], writes=[o.b])
            dst = qT[nch, :, gi * 512:(gi + 1) * 512] if nch < 4 else kT[nch - 4, :, gi * 512:(gi + 1) * 512]
            S.dma(STQ, dst, o[:], o.b, reads=[o.b], final=True)
        psg = pv[n_pv % 2]
        n_pv += 1
        for c in range(8):
            S.op("pe", lambda e, c=c, psg=psg, xT=xT: e.matmul(out=psg[0:16, :], lhsT=wb[:, c, 3072:3088], rhs=xT[:, c, :], start=(c == 0), stop=(c == 7)),
                 reads=[wbufs[c], xT.b], writes=[psg.b], inc=(c == 7))
        og = ogs[gi % 2]
        S.op("act", lambda e, og=og, psg=psg: e.activation(out=og[:], in_=psg[0:16, :], func=AF.Copy), reads=[psg.b], writes=[og.b])
        S.dma("sync", gates[:, gi * 512:(gi + 1) * 512], og[:], og.b, reads=[og.b], final=True)
        for ti in range(4):
            t0 = gi * 512 + ti * 128
            for (c0, c1, kind) in [(512, 1024, "k"), (1024, 1536, "v0"), (1536, 2048, "v1"), (2048, 2560, "o0"), (2560, 3072, "o1")]:
                ps = pv[n_pv % 2]
                n_pv += 1
                nw = c1 - c0
                for c in range(8):
                    S.op("pe", lambda e, c=c, ti=ti, ps=ps, xT=xT, c0=c0, c1=c1, nw=nw: e.matmul(
                        out=ps[:, 0:nw], lhsT=xT[:, c, ti * 128:(ti + 1) * 128], rhs=wb[:, c, c0:c1], start=(c == 0), stop=(c == 7)),
                        reads=[wbufs[c], xT.b], writes=[ps.b], inc=(c == 7))
                if kind in ("k", "v0", "v1"):
                    o = ob[n_ob % 4]
                    n_ob += 1
                    S.op("dve", lambda e, o=o, ps=ps: e.tensor_copy(out=o[:], in_=ps[:]), reads=[ps.b], writes=[o.b])
                    if kind == "k":
                        dst = ktok[t0:t0 + 128, :]
                    else:
                        hf = int(kind[1])
                        dst = v[t0:t0 + 128, hf * 512:(hf + 1) * 512]
                    S.dma(STQ, dst, o[:], o.b, reads=[o.b], final=True)
                else:
                    o = of[n_of % 3]
                    n_of += 1
                    S.op("act", lambda e, o=o, ps=ps, nw=nw: e.activation(out=o[:, 0:nw], in_=ps[:, 0:nw], func=AF.Copy), reads=[ps.b], writes=[o.b])
                    if kind == "g":
                        dst = gates[t0:t0 + 128, :]
                    else:
                        hf = int(kind[1])
                        dst = opre[t0:t0 + 128, hf * 512:(hf + 1) * 512]
                    S.dma(STQ, dst, o[:, 0:nw], o.b, reads=[o.b], final=True)
    S.emit()
    return kb.nc


def build_k5():
    kb = KB()
    S = kb.S
    NCH = SEQ // 64
    qT_d = kb.din("qT", [2, 64, SEQ], BF16)
    kT_d = kb.din("kT", [2, 64, SEQ], BF16)
    ktok_d = kb.din("ktok", [64, NCH, 128], BF16)
    v_d = kb.din("v", [64, NCH, 2, 129], BF16)
    gates_d = kb.din("gates", [64, 4, NCH], F32)
    bg_d = kb.din("bg", [4], F32)
    nh_d = kb.din("nh", [256], F32)
    triu_d = kb.din("triu", [64, 64], F32)
    ones_d = kb.din("ones", [64, 64], F32)
    mask_d = kb.din("mask", [64, 64], BF16)
    hn_d = kb.dout("hn", [SEQ, 256], F32)

    def ld(dram, shape, dt, name, **kw):
        t = kb.sb(shape, dt, name)
        S.dma("sync", t[:], dram, t.b, writes=[t.b], **kw)
        return t
    triu = ld(triu_d, [64, 64], F32, "triu")
    ones = ld(ones_d, [64, 64], F32, "ones")
    mask = ld(mask_d, [64, 64], BF16, "mask")
    G = ld(gates_d, [64, 4, NCH], F32, "G")
    bg = ld(bg_d.partition_broadcast(64), [64, 4], F32, "bg")
    nh = ld(nh_d.partition_broadcast(64), [64, 256], F32, "nh")
    qT = [ld(qT_d[g], [64, SEQ], BF16, "qT") for g in range(2)]
    kT = [ld(kT_d[g], [64, SEQ], BF16, "kT") for g in range(2)]
    ktok = kb.sb([64, NCH, 128], BF16, "ktok")
    vt = kb.sb([64, NCH, 2, 129], BF16, "vt")
    for part in range(4):
        cs = slice(part * 32, (part + 1) * 32)
        S.dma("sync", ktok[:, cs, :], ktok_d[:, cs, :], ktok.b, writes=[ktok.b])
        S.dma("sync", vt[:, cs, :, :], v_d[:, cs, :, :], vt.b, writes=[vt.b])

    b15 = kb.sb([64, 4], F32, "b15")
    S.op("dve", lambda e: e.tensor_scalar(out=b15[:], in0=bg[:], scalar1=1.0 / 15.0, scalar2=None, op0=ALU.mult), reads=[bg.b], writes=[b15.b])
    Tt = kb.sb([64, 4, NCH], F32, "Tt")
    for g4 in range(4):
        S.op("act", lambda e, g4=g4: e.activation(out=Tt[:, g4, :], in_=G[:, g4, :], func=AF.Tanh, scale=1.0 / 15.0, bias=b15[:, g4:g4 + 1]),
             reads=[G.b, b15.b], writes=[Tt.b])
    LF = kb.sb([64, 2, NCH], F32, "LF")
    S.op("act", lambda e: e.activation(out=LF[:], in_=Tt[:, 2:4, :], func=AF.Exp, scale=-15.0), reads=[Tt.b], writes=[LF.b])
    S.op("act", lambda e: e.activation(out=LF[:], in_=LF[:], func=AF.Ln, bias=1.0), reads=[LF.b], writes=[LF.b])
    pnb = kb.ps([64, 2 * NCH], F32, "pnb")
    pne = kb.ps([64, 2 * NCH], F32, "pne")
    S.op("pe", lambda e: e.matmul(out=pnb[:], lhsT=triu[:], rhs=LF[:].rearrange("p g c -> p (g c)"), start=True, stop=True),
         reads=[triu.b, LF.b], writes=[pnb.b])
    S.op("pe", lambda e: e.matmul(out=pne[:], lhsT=ones[:], rhs=LF[:].rearrange("p g c -> p (g c)"), start=True, stop=True),
         reads=[ones.b, LF.b], writes=[pne.b])
    ea = kb.sb([64, 2, NCH], F32, "ea")
    ebt = kb.sb([64, 2, NCH], F32, "ebt")
    ebe = kb.sb([64, 2, NCH], F32, "ebe")
    S.op("dve", lambda e: e.scalar_tensor_tensor(out=ea[:].rearrange("p g c -> p (g c)"), in0=Tt[:, 0:2, :].rearrange("p g c -> p (g c)"),
                                                  scalar=15.0, in1=pnb[:], op0=ALU.mult, op1=ALU.add),
         reads=[Tt.b, pnb.b], writes=[ea.b])
    S.op("act", lambda e: e.activation(out=ea[:], in_=ea[:], func=AF.Exp), reads=[ea.b], writes=[ea.b])
    S.op("act", lambda e: e.activation(out=ebt[:].rearrange("p g c -> p (g c)"), in_=pnb[:], func=AF.Exp, scale=-1.0), reads=[pnb.b], writes=[ebt.b])
    S.op("act", lambda e: e.activation(out=ebe[:].rearrange("p g c -> p (g c)"), in_=pne[:], func=AF.Exp, scale=-1.0), reads=[pne.b], writes=[ebe.b])
    for c in range(NCH):
        for g in range(2):
            eng = "pool" if (c + g) % 2 == 0 else "dve"
            S.op(eng, lambda e, c=c, g=g: e.tensor_scalar(out=vt[:, c, g, :], in0=vt[:, c, g, :], scalar1=ea[:, g, c:c + 1], scalar2=None, op0=ALU.mult),
                 reads=[vt.b, ea.b], writes=[vt.b], inc=(c == NCH - 1))
    for c in range(NCH):
        for g in range(2):
            eng = "dve" if (c + g) % 2 == 0 else "pool"
            S.op(eng, lambda e, c=c, g=g: e.tensor_scalar(out=ktok[:, c, g * 64:(g + 1) * 64], in0=ktok[:, c, g * 64:(g + 1) * 64],
                                                           scalar1=ebe[:, g, c:c + 1], scalar2=None, op0=ALU.mult),
                 reads=[ktok.b, ebe.b], writes=[ktok.b], inc=(c == NCH - 1))

    Cn = [kb.sb([64, 129], F32, "Cn") for _ in range(2)]
    Cb = [[kb.sb([64, 129], BF16, "Cb") for _ in range(2)] for _ in range(2)]
    for g in range(2):
        S.op("pool", lambda e, g=g: e.memset(Cn[g][:], 0.0), writes=[Cn[g].b])
        S.op("pool", lambda e, g=g: e.memset(Cb[g][0][:], 0.0), writes=[Cb[g][0].b])
    NB8 = 8
    stage = [kb.sb([64, NB8, 2, 129], F32, "stage") for _ in range(2)]
    hno = [kb.sb([64, NB8, 256], F32, "hno") for _ in range(2)]
    ssq = [kb.sb([64, NB8 * 2], F32, "ssq") for _ in range(2)]
    den = kb.sb([64, NB8 * 2], F32, "den")
    d2 = kb.sb([64, NB8 * 2], F32, "d2")
    scl = kb.sb([64, NB8 * 2], F32, "scl")
    junk = kb.sb([64, 128], F32, "junk")
    pst = [kb.ps([64, 64], F32, "pst") for _ in range(2)]
    pP = [kb.ps([64, 129], F32, "pP") for _ in range(2)]
    pU = [kb.ps([64, 129], F32, "pU") for _ in range(2)]
    sTs = [kb.sb([64, 64], BF16, "sTs") for _ in range(4)]
    n_s = 0
    for c in range(NCH):
        bi = (c // NB8) % 2
        ci = c % NB8
        cs = slice(c * 64, (c + 1) * 64)
        for g in range(2):
            st_ps = pst[g]
            S.op("pe", lambda e, g=g, cs=cs, st_ps=st_ps: e.matmul(out=st_ps[:], lhsT=kT[g][:, cs], rhs=qT[g][:, cs], start=True, stop=True),
                 reads=[kT[g].b, qT[g].b], writes=[st_ps.b])
            sT = sTs[n_s % 4]
            n_s += 1
            S.op("dve", lambda e, sT=sT, st_ps=st_ps: e.tensor_tensor(out=sT[:], in0=st_ps[:], in1=mask[:], op=ALU.mult),
                 reads=[st_ps.b, mask.b], writes=[sT.b])
            P = pP[g]
            cb_old = Cb[g][c % 2]
            cb_new = Cb[g][(c + 1) % 2]
            S.op("pe", lambda e, P=P, sT=sT, c=c, g=g: e.matmul(out=P[:], lhsT=sT[:], rhs=vt[:, c, g, :], start=True, stop=False),
                 reads=[sT.b, vt.b], writes=[P.b], inc=False)
            S.op("pe", lambda e, P=P, cs=cs, g=g, cb_old=cb_old: e.matmul(out=P[:], lhsT=qT[g][:, cs], rhs=cb_old[:], start=False, stop=True),
                 reads=[qT[g].b, cb_old.b], writes=[P.b])
            U = pU[g]
            S.op("pe", lambda e, U=U, c=c, g=g: e.matmul(out=U[:], lhsT=ktok[:, c, g * 64:(g + 1) * 64], rhs=vt[:, c, g, :], start=True, stop=True),
                 reads=[ktok.b, vt.b], writes=[U.b])
            S.op("dve", lambda e, U=U, g=g, c=c: e.scalar_tensor_tensor(out=Cn[g][:], in0=Cn[g][:], scalar=ebe[:, g, c:c + 1], in1=U[:],
                                                                       op0=ALU.mult, op1=ALU.add),
                 reads=[Cn[g].b, ebe.b, U.b], writes=[Cn[g].b])
            S.op("act", lambda e, g=g, cb_new=cb_new: e.activation(out=cb_new[:], in_=Cn[g][:], func=AF.Copy),
                 reads=[Cn[g].b], writes=[cb_new.b])
            S.op("act", lambda e, P=P, bi=bi, ci=ci, g=g, c=c: e.activation(out=stage[bi][:, ci, g, :], in_=P[:], func=AF.Copy, scale=ebt[:, g, c:c + 1]),
                 reads=[P.b, ebt.b], writes=[stage[bi].b])
        if ci == NB8 - 1:
            st = stage[bi]
            ho = hno[bi]
            sq = ssq[bi]
            for i in range(NB8):
                for g in range(2):
                    S.op("act", lambda e, st=st, sq=sq, i=i, g=g: e.activation(out=junk[:], in_=st[:, i, g, 0:128], func=AF.Square,
                                                                              accum_out=sq[:, i * 2 + g:i * 2 + g + 1]),
                         reads=[st.b], writes=[junk.b, sq.b])
            stf = st[:].rearrange("p i g d -> p (i g) d")
            S.op("dve", lambda e, stf=stf: e.tensor_tensor(out=d2[:], in0=stf[:, :, 128], in1=stf[:, :, 128], op=ALU.mult),
                 reads=[st.b], writes=[d2.b])
            S.op("dve", lambda e: e.tensor_scalar(out=d2[:], in0=d2[:], scalar1=1.0, scalar2=None, op0=ALU.max), reads=[d2.b], writes=[d2.b])
            S.op("dve", lambda e, sq=sq: e.scalar_tensor_tensor(out=d2[:], in0=d2[:], scalar=EPS * 128.0, in1=sq[:], op0=ALU.mult, op1=ALU.add),
                 reads=[d2.b, sq.b], writes=[d2.b])
            S.op("act", lambda e: e.activation(out=scl[:], in_=d2[:], func=AF.Ln, scale=1.0 / 128.0), reads=[d2.b], writes=[scl.b])
            S.op("act", lambda e: e.activation(out=scl[:], in_=scl[:], func=AF.Exp, scale=-0.5), reads=[scl.b], writes=[scl.b])
            for i in range(NB8):
                for g in range(2):
                    S.op("dve", lambda e, st=st, ho=ho, i=i, g=g: e.scalar_tensor_tensor(
                        out=ho[:, i, g * 128:(g + 1) * 128], in0=st[:, i, g, 0:128], scalar=scl[:, i * 2 + g:i * 2 + g + 1],
                        in1=nh[:, g * 128:(g + 1) * 128], op0=ALU.mult, op1=ALU.mult),
                        reads=[st.b, scl.b, nh.b], writes=[ho.b])
            c0 = c - (NB8 - 1)
            S.dma(STQ, hn_d[c0 * 64:(c0 + NB8) * 64, :].rearrange("(c s) f -> s c f", s=64), ho[:], ho.b, reads=[ho.b], final=True)
    S.emit()
    return kb.nc


def k5_maps(q, k, ktokf, v, gates, bgates, normh):
    triu = (np.arange(64)[:, None] <= np.arange(64)[None, :])
    maps = []
    for core in range(NCORE):
        b, p = core // 4, core % 4
        hs = [2 * p, 2 * p + 1]
        m = {}
        m["qT"] = np.ascontiguousarray(np.stack([q[b][:, h * 64:(h + 1) * 64].T for h in hs]))
        m["kT"] = np.ascontiguousarray(np.stack([k[b][:, h * 64:(h + 1) * 64].T for h in hs]))
        m["ktok"] = np.ascontiguousarray(ktokf[b][:, 128 * p:128 * p + 128].reshape(128, 64, 128).transpose(1, 0, 2))
        vv = v[b][:, 256 * p:256 * p + 256].reshape(128, 64, 2, 128).transpose(1, 0, 2, 3)
        m["v"] = np.ascontiguousarray(np.concatenate([vv, np.ones((64, 128, 2, 1), vv.dtype)], axis=3))
        cols = [hs[0], hs[1], 8 + hs[0], 8 + hs[1]]
        m["gates"] = np.ascontiguousarray(gates[b][:, cols].reshape(128, 64, 4).transpose(1, 2, 0))
        m["bg"] = np.ascontiguousarray(bgates[cols])
        m["nh"] = np.ascontiguousarray(normh[256 * p:256 * p + 256])
        m["triu"] = triu.astype(np.float32)
        m["ones"] = np.ones((64, 64), np.float32)
        m["mask"] = triu.astype(NPBF)
        maps.append(m)
    return maps


_CACHE = {}


def _get(name, fn, *a):
    key = (name,) + a
    if key not in _CACHE:
        _CACHE[key] = fn(*a)
    return _CACHE[key]


IDENT = np.eye(128).astype(NPBF)


def run(nc, in_maps):
    res = run_bass_kernel_spmd(nc, in_maps, core_ids=list(range(NCORE)))
    return res.results


def _tok_shards(a):
    F = a.shape[-1]
    return list(np.ascontiguousarray(a).reshape(NCORE, TOK, F))


def kernel(x, norm_mix_pre, norm_mix_post, norm_ffn_pre, norm_ffn_post, w_up, w_down,
           attn_w_qkv, attn_w_o, mlstm_w_in, mlstm_b_gates, mlstm_norm_h, mlstm_w_out):
    f32 = np.float32
    x = np.asarray(x, f32)
    A = lambda a: np.ascontiguousarray(np.asarray(a, f32))
    xs = _tok_shards(x)
    nc1 = _get("k1", build_k1)
    r1 = run(nc1, [{"x": xs[i], "g": A(norm_mix_pre[0]), "w": A(attn_w_qkv[0]), "ident": IDENT} for i in range(NCORE)])
    maps = []
    for core in range(NCORE):
        b, hp = core // 4, core % 4
        hs = [2 * hp, 2 * hp + 1]
        m = {}
        m["qT"] = np.ascontiguousarray(np.concatenate([r1[b * 4 + r]["qT"][hs[0]:hs[0] + 2] for r in range(4)], axis=2))
        m["kT"] = np.ascontiguousarray(np.concatenate([r1[b * 4 + r]["kT"][hs[0]:hs[0] + 2] for r in range(4)], axis=2))
        vb = np.concatenate([r1[b * 4 + r]["v"] for r in range(4)], axis=0)
        vv = []
        for h in hs:
            t = vb[:, h * 128:(h + 1) * 128].reshape(64, 128, 128).transpose(1, 0, 2)
            vv.append(np.concatenate([t, np.ones((128, 64, 1), t.dtype)], axis=2))
        m["v"] = np.ascontiguousarray(np.stack(vv))
        m.update(k2_consts(hs))
        maps.append(m)
    nc2 = _get("k2", build_k2)
    r2 = run(nc2, maps)
    o_full = np.zeros((NB, SEQ, D), f32)
    for core in range(NCORE):
        b, hp = core // 4, core % 4
        for i, h in enumerate([2 * hp, 2 * hp + 1]):
            o_full[b][:, h * 128:(h + 1) * 128] = r2[core]["o"][i]
    os_ = _tok_shards(o_full)
    nc3a = _get("k3a", build_k3a, "attn")
    r3 = run(nc3a, [{"a_in": os_[i], "h_in": xs[i], "w": A(attn_w_o[0]), "gpost": A(norm_mix_post[0]), "ident": IDENT} for i in range(NCORE)])
    nc3b = _get("k3b", build_k3b)
    r4 = run(nc3b, [{"h_in": r3[i]["h_out"], "gpre": A(norm_ffn_pre[0]), "w_up": A(w_up[0]), "w_dn": A(w_down[0]),
                     "gpost": A(norm_ffn_post[0]), "ident": IDENT} for i in range(NCORE)])
    hs0 = [r4[i]["h_out"] for i in range(NCORE)]
    nc4 = _get("k4", build_k4)
    r5 = run(nc4, [{"x": hs0[i], "g": A(norm_mix_pre[1]), "w": A(mlstm_w_in[0]), "ident": IDENT} for i in range(NCORE)])
    triu = (np.arange(64)[:, None] <= np.arange(64)[None, :])
    bgates = A(mlstm_b_gates[0])
    normh = A(mlstm_norm_h[0])
    maps = []
    for core in range(NCORE):
        b, p = core // 4, core % 4
        hs = [2 * p, 2 * p + 1]
        m = {}
        m["qT"] = np.ascontiguousarray(np.concatenate([r5[b * 4 + r]["qT"][p] for r in range(4)], axis=1).reshape(2, 64, SEQ))
        m["kT"] = np.ascontiguousarray(np.concatenate([r5[b * 4 + r]["kT"][p] for r in range(4)], axis=1).reshape(2, 64, SEQ))
        kt = np.concatenate([r5[b * 4 + r]["ktok"][:, 128 * p:128 * p + 128] for r in range(4)], axis=0)
        m["ktok"] = np.ascontiguousarray(kt.reshape(128, 64, 128).transpose(1, 0, 2))
        vb = np.concatenate([r5[b * 4 + r]["v"][:, 256 * p:256 * p + 256] for r in range(4)], axis=0)
        vv = vb.reshape(128, 64, 2, 128).transpose(1, 0, 2, 3)
        m["v"] = np.ascontiguousarray(np.concatenate([vv, np.ones((64, 128, 2, 1), vv.dtype)], axis=3))
        gt = np.concatenate([r5[b * 4 + r]["gatesT"] for r in range(4)], axis=1).T
        cols = [hs[0], hs[1], 8 + hs[0], 8 + hs[1]]
        m["gates"] = np.ascontiguousarray(gt[:, cols].reshape(128, 64, 4).transpose(1, 2, 0))
        m["bg"] = np.ascontiguousarray(bgates[cols])
        m["nh"] = np.ascontiguousarray(normh[256 * p:256 * p + 256])
        m["triu"] = triu.astype(f32)
        m["ones"] = np.ones((64, 64), f32)
        m["mask"] = triu.astype(NPBF)
        maps.append(m)
    nc5 = _get("k5", build_k5)
    r6 = run(nc5, maps)
    hn_full = np.zeros((NB, SEQ, D), f32)
    for core in range(NCORE):
        b, p = core // 4, core % 4
        hn_full[b][:, 256 * p:256 * p + 256] = r6[core]["hn"]
    hns = _tok_shards(hn_full)
    nc6 = _get("k3a", build_k3a, "mlstm")
    r7 = run(nc6, [{"a_in": hns[i], "opre": r5[i]["opre"], "h_in": hs0[i], "w": A(mlstm_w_out[0]), "gpost": A(norm_mix_post[1]),
                    "ident": IDENT} for i in range(NCORE)])
    r8 = run(nc3b, [{"h_in": r7[i]["h_out"], "gpre": A(norm_ffn_pre[1]), "w_up": A(w_up[1]), "w_dn": A(w_down[1]),
                     "gpost": A(norm_ffn_post[1]), "ident": IDENT} for i in range(NCORE)])
    out = np.stack([r8[i]["h_out"] for i in range(NCORE)]).reshape(NB, SEQ, D).astype(f32)
    return out
```
